# Optimizing a Trainium2 kernel written in Bass

```python
import jax, jax.numpy as jnp
from jax import lax
import numpy as np

D_MODEL = 1024
BATCH = 8
SEQ = 2048
DEPTH = 4

RET_HEADS = 4
RET_HEAD_DIM = 128
RET_W = RET_HEADS * RET_HEAD_DIM
RET_CHUNK = 128
CONV_CH = 512
CONV_WIDTH = 31
MOBA_HEADS = 8
MOBA_HEAD_DIM = 64
MOBA_W = MOBA_HEADS * MOBA_HEAD_DIM
MOBA_BLOCK = 256
MOBA_TOPK = 3
MOBA_Q_CHUNK = 64
ROPE_THETA = 10000.0
N_BRANCH = 3
IN_COLS = 4 * RET_W + 2 * CONV_CH + 3 * MOBA_W + N_BRANCH * D_MODEL
FF_DENSE = 2816
N_EXPERTS = 8
TOP_K = 2
FF_EXPERT = 3584
MOE_GROUP = 256
NORM_EPS = 1e-6

kernel_name = 'hybrid_retention_conformer_moba_moe_block'


def rms_norm(x, g):
    x32 = x.astype(jnp.float32)
    y = x32 * lax.rsqrt(jnp.mean(x32 * x32, axis=-1, keepdims=True) + NORM_EPS)
    return (y * g.astype(jnp.float32)).astype(x.dtype)


def layer_norm(x, g, b):
    x32 = x.astype(jnp.float32)
    mu = jnp.mean(x32, axis=-1, keepdims=True)
    var = jnp.mean(jnp.square(x32 - mu), axis=-1, keepdims=True)
    y = (x32 - mu) * lax.rsqrt(var + NORM_EPS)
    return (y * g.astype(jnp.float32) + b.astype(jnp.float32)).astype(x.dtype)


def rope_tables(T, d):
    inv = 1.0 / (ROPE_THETA ** (jnp.arange(0, d, 2, dtype=jnp.float32) / d))
    ang = jnp.arange(T, dtype=jnp.float32)[:, None] * inv[None, :]
    return jnp.cos(ang), jnp.sin(ang)


def apply_rope(x, cos, sin):
    x32 = x.astype(jnp.float32)
    half = x.shape[-1] // 2
    x1, x2 = x32[..., :half], x32[..., half:]
    c = cos[None, :, None, :]
    s = sin[None, :, None, :]
    return jnp.concatenate([x1 * c - x2 * s, x2 * c + x1 * s], axis=-1).astype(x.dtype)


def retention_chunkwise(q, k, v):
    B, T, H, d = q.shape
    C = RET_CHUNK
    nC = T // C
    q, k, v = (t.astype(jnp.float32).reshape(B, nC, C, H, d) for t in (q, k, v))
    log_g = jnp.log(1.0 - jnp.exp2(-5.0 - jnp.arange(H, dtype=jnp.float32)))
    pos = jnp.arange(C, dtype=jnp.float32)
    diff = pos[:, None] - pos[None, :]
    decay_intra = jnp.where(diff[None] >= 0, jnp.exp(diff[None] * log_g[:, None, None]), 0.0)
    scores = jnp.einsum('bcnhd,bcmhd->bchnm', q, k) * decay_intra[None, None]
    y_intra = jnp.einsum('bchnm,bcmhd->bcnhd', scores, v)
    w_state = jnp.exp((C - 1.0 - pos)[None, :] * log_g[:, None])
    kv = jnp.einsum('bcmhd,bcmhe,hm->bchde', k, v, w_state)
    g_chunk = jnp.exp(C * log_g)[None, :, None, None]

    def step(S, kv_c):
        return g_chunk * S + kv_c, S

    _, S_prev = lax.scan(step, jnp.zeros((B, H, d, d), jnp.float32), jnp.moveaxis(kv, 1, 0))
    S_prev = jnp.moveaxis(S_prev, 0, 1)
    w_cross = jnp.exp((pos + 1.0)[:, None] * log_g[None, :])
    y_cross = jnp.einsum('bcnhd,bchde->bcnhe', q, S_prev) * w_cross[None, None, :, :, None]
    return (y_intra + y_cross).reshape(B, T, H, d)


def head_group_norm(y):
    mu = jnp.mean(y, axis=-1, keepdims=True)
    var = jnp.mean(jnp.square(y - mu), axis=-1, keepdims=True)
    return (y - mu) * lax.rsqrt(var + 1e-5)


def conformer_conv(ca, cb, conv_w, conv_b, ln_g, ln_b):
    u = ca * jax.nn.sigmoid(cb)
    y = lax.conv_general_dilated(
        u, conv_w[:, None, :].astype(u.dtype), window_strides=(1,),
        padding=[(CONV_WIDTH - 1, 0)], dimension_numbers=('NWC', 'WIO', 'NWC'),
        feature_group_count=CONV_CH) + conv_b.astype(u.dtype)
    return jax.nn.silu(layer_norm(y, ln_g, ln_b))


def moba_attention(q, k, v):
    B, T, H, d = q.shape
    BS = MOBA_BLOCK
    nB = -(-T // BS)
    pad = nB * BS - T
    q, k, v = (t.transpose(0, 2, 1, 3) for t in (q, k, v))
    kb = jnp.pad(k, ((0, 0), (0, 0), (0, pad), (0, 0))).reshape(B, H, nB, BS, d)
    vb = jnp.pad(v, ((0, 0), (0, 0), (0, pad), (0, 0))).reshape(B, H, nB, BS, d)
    n_in_block = jnp.clip(T - jnp.arange(nB) * BS, 1, BS).astype(jnp.float32)
    k_mean = kb.astype(jnp.float32).sum(axis=3) / n_in_block[:, None]
    gate = jnp.einsum('bhtd,bhnd->bhtn', q.astype(jnp.float32), k_mean)
    q_blk = jnp.arange(T) // BS
    past = jnp.arange(nB)[None, :] < q_blk[:, None]
    gate = jnp.where(past[None, None], gate, -jnp.inf)
    ksel = max(1, min(MOBA_TOPK, nB - 1))
    _, idx = lax.top_k(gate, ksel)
    valid = idx < q_blk[None, None, :, None]
    scale = d ** -0.5
    bi = jnp.arange(B)[:, None, None, None]
    hi = jnp.arange(H)[None, :, None, None]

    def chunk(c):
        t0 = c * MOBA_Q_CHUNK
        qc = lax.dynamic_slice_in_dim(q, t0, MOBA_Q_CHUNK, axis=2)
        idc = lax.dynamic_slice_in_dim(idx, t0, MOBA_Q_CHUNK, axis=2)
        vac = lax.dynamic_slice_in_dim(valid, t0, MOBA_Q_CHUNK, axis=2)
        k_sel = kb[bi, hi, idc]
        s_sel = jnp.einsum('bhqd,bhqkld->bhqkl', qc, k_sel).astype(jnp.float32) * scale
        s_sel = jnp.where(vac[..., None], s_sel, -jnp.inf).reshape(B, H, MOBA_Q_CHUNK, ksel * BS)
        ob = t0 // BS
        k_own = lax.dynamic_index_in_dim(kb, ob, axis=2, keepdims=False)
        v_own = lax.dynamic_index_in_dim(vb, ob, axis=2, keepdims=False)
        s_own = jnp.einsum('bhqd,bhld->bhql', qc, k_own).astype(jnp.float32) * scale
        k_pos = ob * BS + jnp.arange(BS)
        q_pos = t0 + jnp.arange(MOBA_Q_CHUNK)
        s_own = jnp.where((k_pos[None, :] <= q_pos[:, None])[None, None], s_own, -jnp.inf)
        p = jax.nn.softmax(jnp.concatenate([s_sel, s_own], axis=-1), axis=-1).astype(q.dtype)
        p_sel = p[..., :ksel * BS].reshape(B, H, MOBA_Q_CHUNK, ksel, BS)
        p_own = p[..., ksel * BS:]
        v_sel = vb[bi, hi, idc]
        return (jnp.einsum('bhqkl,bhqkld->bhqd', p_sel, v_sel)
                + jnp.einsum('bhql,bhld->bhqd', p_own, v_own))

    out = lax.map(chunk, jnp.arange(T // MOBA_Q_CHUNK))
    return out.transpose(1, 0, 3, 2, 4).reshape(B, T, H * d)


def token_mixer(h, w_in, conv_w, conv_b, conv_ln_g, conv_ln_b, q_norm_g, k_norm_g,
                w_ret_o, w_conv_o, w_moba_o, w_out, cos_r, sin_r, cos_m, sin_m):
    B, T, _ = h.shape
    proj = h @ w_in
    sizes = (RET_W,) * 4 + (CONV_CH,) * 2 + (MOBA_W,) * 3 + (D_MODEL,) * N_BRANCH
    points = np.cumsum(sizes)[:-1].tolist()
    rq, rk, rv, rg, ca, cb, mq, mk, mv, gr, gc, gm = jnp.split(proj, points, axis=-1)

    q = apply_rope(rq.reshape(B, T, RET_HEADS, RET_HEAD_DIM), cos_r, sin_r)
    k = apply_rope(rk.reshape(B, T, RET_HEADS, RET_HEAD_DIM), cos_r, sin_r) * (RET_HEAD_DIM ** -0.5)
    v = rv.reshape(B, T, RET_HEADS, RET_HEAD_DIM)
    y = head_group_norm(retention_chunkwise(q, k, v)).reshape(B, T, RET_W).astype(h.dtype)
    y_ret = (jax.nn.silu(rg) * y) @ w_ret_o

    y_conv = conformer_conv(ca, cb, conv_w, conv_b, conv_ln_g, conv_ln_b) @ w_conv_o

    q = apply_rope(rms_norm(mq.reshape(B, T, MOBA_HEADS, MOBA_HEAD_DIM), q_norm_g), cos_m, sin_m)
    k = apply_rope(rms_norm(mk.reshape(B, T, MOBA_HEADS, MOBA_HEAD_DIM), k_norm_g), cos_m, sin_m)
    v = mv.reshape(B, T, MOBA_HEADS, MOBA_HEAD_DIM)
    y_moba = moba_attention(q, k, v) @ w_moba_o

    merged = (jax.nn.sigmoid(gr) * y_ret + jax.nn.sigmoid(gc) * y_conv
              + jax.nn.sigmoid(gm) * y_moba)
    return merged @ w_out


def swiglu(h, w_gate, w_up, w_down):
    return (jax.nn.silu(h @ w_gate) * (h @ w_up)) @ w_down


def moe_swiglu(h, w_router, w_e_gate, w_e_up, w_e_down):
    B, T, D = h.shape
    N = B * T
    NA = N * TOP_K
    xf = h.reshape(N, D)
    logits = (xf @ w_router).astype(jnp.float32)
    top_vals, top_idx = lax.top_k(logits, TOP_K)
    gates = jax.nn.softmax(top_vals, axis=-1)
    e_flat = top_idx.reshape(-1)
    tok = jnp.arange(NA) // TOP_K
    order = jnp.argsort(e_flat)
    se, stok, sg = e_flat[order], tok[order], gates.reshape(-1)[order]
    counts = jnp.bincount(e_flat, length=N_EXPERTS)
    padded = ((counts + MOE_GROUP - 1) // MOE_GROUP) * MOE_GROUP
    pad_end = jnp.cumsum(padded)
    pad_start = pad_end - padded
    grp_start = jnp.cumsum(counts) - counts
    dest = pad_start[se] + (jnp.arange(NA) - grp_start[se])
    P = (-(-NA // MOE_GROUP) + N_EXPERTS) * MOE_GROUP
    n_grp = P // MOE_GROUP
    xbuf = jnp.zeros((P, D), xf.dtype).at[dest].set(xf[stok])
    grp_expert = jnp.minimum(
        jnp.searchsorted(pad_end, jnp.arange(n_grp) * MOE_GROUP, side='right'), N_EXPERTS - 1)

    def expert_group(args):
        xg, e = args
        return (jax.nn.silu(xg @ w_e_gate[e]) * (xg @ w_e_up[e])) @ w_e_down[e]

    ybuf = lax.map(expert_group, (xbuf.reshape(n_grp, MOE_GROUP, D), grp_expert)).reshape(P, D)
    y = ybuf[dest] * sg[:, None].astype(ybuf.dtype)
    out = jnp.zeros((N, D), ybuf.dtype).at[stok].add(y)
    return out.reshape(B, T, D)


def setup_inputs(seed: int = 0) -> dict:
    key = jax.random.key(seed)
    ks = jax.random.split(key, 21)
    f32 = jnp.float32
    n_dense = (DEPTH + 1) // 2
    n_moe = DEPTH // 2
    res_scale = (2 * DEPTH) ** -0.5

    def nrm(k, shape, scale):
        return jax.random.normal(k, shape, f32) * scale

    return {
        'x': nrm(ks[0], (BATCH, SEQ, D_MODEL), 1.0),
        'g_mix': 1.0 + nrm(ks[1], (DEPTH, D_MODEL), 0.01),
        'w_in': nrm(ks[2], (DEPTH, D_MODEL, IN_COLS), D_MODEL ** -0.5),
        'conv_w': nrm(ks[3], (DEPTH, CONV_WIDTH, CONV_CH), CONV_WIDTH ** -0.5),
        'conv_b': nrm(ks[4], (DEPTH, CONV_CH), 0.01),
        'conv_ln_g': 1.0 + nrm(ks[5], (DEPTH, CONV_CH), 0.01),
        'conv_ln_b': nrm(ks[6], (DEPTH, CONV_CH), 0.01),
        'q_norm_g': 1.0 + nrm(ks[7], (DEPTH, MOBA_HEAD_DIM), 0.01),
        'k_norm_g': 1.0 + nrm(ks[8], (DEPTH, MOBA_HEAD_DIM), 0.01),
        'w_ret_o': nrm(ks[9], (DEPTH, RET_W, D_MODEL), RET_W ** -0.5),
        'w_conv_o': nrm(ks[10], (DEPTH, CONV_CH, D_MODEL), CONV_CH ** -0.5),
        'w_moba_o': nrm(ks[11], (DEPTH, MOBA_W, D_MODEL), MOBA_W ** -0.5),
        'w_out': nrm(ks[12], (DEPTH, D_MODEL, D_MODEL), D_MODEL ** -0.5 * res_scale),
        'g_ffn': 1.0 + nrm(ks[13], (DEPTH, D_MODEL), 0.01),
        'w_ff_gate': nrm(ks[14], (n_dense, D_MODEL, FF_DENSE), D_MODEL ** -0.5),
        'w_ff_up': nrm(ks[15], (n_dense, D_MODEL, FF_DENSE), D_MODEL ** -0.5),
        'w_ff_down': nrm(ks[16], (n_dense, FF_DENSE, D_MODEL), FF_DENSE ** -0.5 * res_scale),
        'w_router': nrm(ks[17], (n_moe, D_MODEL, N_EXPERTS), D_MODEL ** -0.5),
        'w_e_gate': nrm(ks[18], (n_moe, N_EXPERTS, D_MODEL, FF_EXPERT), D_MODEL ** -0.5),
        'w_e_up': nrm(ks[19], (n_moe, N_EXPERTS, D_MODEL, FF_EXPERT), D_MODEL ** -0.5),
        'w_e_down': nrm(ks[20], (n_moe, N_EXPERTS, FF_EXPERT, D_MODEL), FF_EXPERT ** -0.5 * res_scale),
    }


def reference(x, g_mix, w_in, conv_w, conv_b, conv_ln_g, conv_ln_b, q_norm_g, k_norm_g,
              w_ret_o, w_conv_o, w_moba_o, w_out, g_ffn, w_ff_gate, w_ff_up, w_ff_down,
              w_router, w_e_gate, w_e_up, w_e_down):
    T = x.shape[1]
    cos_r, sin_r = rope_tables(T, RET_HEAD_DIM)
    cos_m, sin_m = rope_tables(T, MOBA_HEAD_DIM)
    for l in range(DEPTH):
        h = rms_norm(x, g_mix[l])
        x = x + token_mixer(h, w_in[l], conv_w[l], conv_b[l], conv_ln_g[l], conv_ln_b[l],
                            q_norm_g[l], k_norm_g[l], w_ret_o[l], w_conv_o[l], w_moba_o[l],
                            w_out[l], cos_r, sin_r, cos_m, sin_m)
        h = rms_norm(x, g_ffn[l])
        j = l // 2
        if l % 2 == 0:
            x = x + swiglu(h, w_ff_gate[j], w_ff_up[j], w_ff_down[j])
        else:
            x = x + moe_swiglu(h, w_router[j], w_e_gate[j], w_e_up[j], w_e_down[j])
    return x
```

```python
import numpy as np
from contextlib import ExitStack
RS = 99
R2 = 99
R3 = 0
import concourse.bass as bass
import concourse.mybir as mybir
from concourse.bass_utils import run_bass_kernel_spmd

F32 = mybir.dt.float32
BF16 = mybir.dt.bfloat16
AF = mybir.ActivationFunctionType
ALU = mybir.AluOpType
AX = mybir.AxisListType

T = 2048
D = 1024
L = 4
NTC = 4
EPS = 1e-6
ENGS = ("pe", "act", "dve", "pool", "sp")

CB_ID, CB_OD, CB_O512, CB_O128, CB_B64, CB_RR, CB_RM, CB_TRI = [i * 128 for i in range(8)]
CB_N = 1024
CF_DEC, CF_WCB, CF_WS, CF_IDF, CF_PNEG, CF_PMSK, CF_SEL8 = 0, 512, 1024, 1028, 1156, 1412, 1668
CF_N = 1668 + 1024
V_GM, V_GF, V_CW, V_CB, V_LG, V_LB, V_QG, V_KG = 0, 8, 16, 140, 144, 148, 152, 153
V_N = 154


class Prog:
    def __init__(self, nc, es):
        self.nc = nc
        self.es = es
        self.ops = {e: [] for e in ENGS}
        self.cnt = {e: 0 for e in ENGS}
        self.esem = {e: es.enter_context(nc.semaphore("se_" + e)) for e in ENGS if e != "sp"}
        self.known = {e: {} for e in ENGS}
        self.res = {}
        self.dsem = {}
        self.dcnt = {}
        self.nbank = 0

    def _deps(self, reads, writes):
        deps = []
        for k in reads:
            r = self.res.get(k)
            if r is not None and r["w"] is not None:
                deps.append(r["w"])
        for k in writes:
            r = self.res.get(k)
            if r is not None:
                if r["w"] is not None:
                    deps.append(r["w"])
                deps.extend(r["r"].values())
        return deps

    def _resolve(self, eng, deps):
        waits = []
        kn = self.known[eng]
        for (kind, key, val) in deps:
            if kind == "E" and key == eng and eng in ("pe", "sp"):
                continue
            kk = (kind, key)
            if kn.get(kk, 0) >= val:
                continue
            kn[kk] = val
            waits.append((kind, key, val))
        return waits

    def _commit(self, tok, reads, writes):
        for k in reads:
            r = self.res.setdefault(k, {"w": None, "r": {}})
            r["r"][(tok[0], tok[1])] = tok
        for k in writes:
            self.res[k] = {"w": tok, "r": {}}

    def op(self, eng, fn, reads=(), writes=()):
        waits = self._resolve(eng, self._deps(reads, writes))
        self.cnt[eng] += 1
        tok = ("E", eng, self.cnt[eng])
        self.ops[eng].append((waits, fn, ("E", eng)))
        self._commit(tok, reads, writes)

    def dma(self, q, out, in_, semkey, reads=(), writes=()):
        waits = self._resolve(q, self._deps(reads, writes))
        if semkey not in self.dsem:
            self.dsem[semkey] = self.es.enter_context(self.nc.semaphore("sd_" + str(len(self.dsem))))
            self.dcnt[semkey] = 0
        self.dcnt[semkey] += 16
        tok = ("D", semkey, self.dcnt[semkey])
        rs = lambda a: a() if callable(a) else a
        self.ops[q].append((waits, lambda e: e.dma_start(out=rs(out), in_=rs(in_)), ("D", semkey)))
        self._commit(tok, reads, writes)
        return tok

    def barrier(self):
        toks = [("E", e, self.cnt[e]) for e in ENGS if e != "sp" and self.cnt[e] > 0]
        toks += [("D", k, v) for k, v in self.dcnt.items()]
        for f in ENGS:
            waits = self._resolve(f, toks)
            if waits:
                self.ops[f].append((waits, None, None))

    def wait_tok(self, eng, tok):
        waits = self._resolve(eng, [tok])
        if waits:
            self.ops[eng].append((waits, None, None))

    def emit(self, eng, e):
        for waits, fn, inc in self.ops[eng]:
            for (kind, key, val) in waits:
                s = self.esem[key] if kind == "E" else self.dsem[key]
                e.wait_ge(s, val)
            if fn is None:
                continue
            ins = fn(e)
            if inc[0] == "E":
                ins.then_inc(self.esem[inc[1]], 1)
            else:
                ins.then_inc(self.dsem[inc[1]], 16)


def build_nc(layers=(0, 1, 2, 3), dump=None, phases=("ret", "conv", "moba", "merge", "ffn"), nbatch=1):
    nc = bass.Bass("TRN2", target_bir_lowering=False)

    def din(name, shape):
        return nc.dram_tensor(name, list(shape), F32, kind="ExternalInput").ap()

    xT_d = din("xT", [nbatch, D, T])
    w_in_d = din("w_in", [L, 60, 128, 1024])
    w_bo_d = din("w_bo", [L, 3, 8, 128, 512])
    w_out_d = din("w_out", [L, 8, 128, 1024])
    w_fg_d = din("w_fg", [2, 22, 128, 1024])
    w_fu_d = din("w_fu", [2, 22, 128, 1024])
    w_fd_d = din("w_fd", [2, 22, 128, 1024])
    has_moe = any(l % 2 == 1 for l in layers)
    w_eg_d = w_eu_d = w_ed_d = None
    if has_moe:
        w_eg_d = din("w_eg", [2, 8, 28, 128, 1024])
        w_eu_d = din("w_eu", [2, 8, 28, 128, 1024])
        w_ed_d = din("w_ed", [2, 8, 28, 128, 1024])
    w_rt_d = din("w_rt", [2, 128, 64])
    vec_d = din("vec", [128, L * V_N])
    cbf_d = din("cbf", [128, CB_N])
    rope_d = din("rope", [2, 128, 2 * T])
    cf_d = din("cf", [128, CF_N])
    out_d = nc.dram_tensor("outT", [nbatch, D, T], F32, kind="ExternalOutput").ap()
    dump_d = None
    if dump is not None:
        dump_d = nc.dram_tensor("dump", [128, dump], F32, kind="ExternalOutput").ap()

    es = ExitStack()
    with es:
        def sb(name, shape, dt):
            return es.enter_context(nc.sbuf_tensor(name, list(shape), dt))

        xT = sb("xT_s", [128, 8, T], F32)
        hT = sb("hT_s", [128, 8, T], BF16)
        cbf = sb("cbf_s", [128, CB_N], BF16)
        rope = sb("rope_s", [128, 2, T], BF16)
        cf = sb("cf_s", [128, CF_N], F32)
        vec = sb("vec_s", [128, L * V_N], F32)
        ARN = 45440
        arena = sb("arena", [128, ARN], BF16)
        psum = [es.enter_context(nc.psum_tensor("ps%d" % i, [128, 512], F32)) for i in range(8)]

        P = Prog(nc, es)

        held = set()

        def bank(hold=False):
            while True:
                b = P.nbank % 8
                P.nbank += 1
                if b not in held:
                    break
            if hold:
                held.add(b)
            return b

        class Ar:
            def __init__(self):
                self.off = 0

            def bf(self, n):
                a = arena[:, self.off:self.off + n]
                self.off += n
                assert self.off <= ARN, self.off
                return a

            def f32(self, n):
                return self.bf(2 * n).bitcast(F32)

        def ident():
            return cbf[:, CB_ID:CB_ID + 128]

        def mm(out, lhsT, rhs, start, stop, reads, writes):
            P.op("pe", lambda e: e.matmul(out, lhsT, rhs, start=start, stop=stop), reads, writes)

        def tr(out, in_, reads, writes):
            P.op("pe", lambda e: e.transpose(out, in_, ident()), list(reads) + ["cbf"], writes)

        def actf(out, in_, func, reads, writes, scale=1.0, bias=None):
            if bias is None:
                P.op("act", lambda e: e.activation(out=out, in_=in_, func=func, scale=scale), reads, writes)
            else:
                P.op("act", lambda e: e.activation(out=out, in_=in_, func=func, scale=scale, bias=bias), reads, writes)

        def tt(eng, out, in0, in1, op, reads, writes):
            P.op(eng, lambda e: e.tensor_tensor(out=out, in0=in0, in1=in1, op=op), reads, writes)

        def ts(eng, out, in0, s1, s2, op0, op1, reads, writes):
            if s2 is None:
                P.op(eng, lambda e: e.tensor_scalar(out=out, in0=in0, scalar1=s1, scalar2=None, op0=op0), reads, writes)
            else:
                P.op(eng, lambda e: e.tensor_scalar(out=out, in0=in0, scalar1=s1, scalar2=s2, op0=op0, op1=op1), reads, writes)

        def stt(out, in0, scalar, in1, op0, op1, reads, writes):
            P.op("dve", lambda e: e.scalar_tensor_tensor(out=out, in0=in0, scalar=scalar, in1=in1, op0=op0, op1=op1), reads, writes)

        def recip(out, in_, reads, writes):
            P.op("dve", lambda e: e.reciprocal(out=out, in_=in_), reads, writes)

        def wload(dst, src, key):
            P.dma("pool", dst, src, key, reads=(), writes=[key])

        def tsl(tc):
            return slice(tc * 512, (tc + 1) * 512)

        def vcol(l, off, n=1):
            return vec[:, l * V_N + off:l * V_N + off + n]

        P.dma("sp", vec[:], vec_d, "ld_v", writes=["vec"])
        P.dma("sp", cf[:], cf_d, "ld_cf", writes=["cf"])
        P.dma("pool", cbf[:], cbf_d, "ld_cb", writes=["cbf"])

        XR = lambda tc: [("x", c, tc) for c in range(8)]
        HR = lambda tc: [("h", tc)]

        def rmsnorm(l, goff, ar, hf=None):
            sq = ar.bf(8 * 512).rearrange("p (c t) -> p c t", c=8)
            rs = ar.f32(512)
            for tc in range(NTC):
                s = tsl(tc)
                actf(sq, xT[:, :, s], AF.Square, XR(tc), ["sq"])
                b = bank()
                for c in range(8):
                    mm(psum[b][:], cbf[:, CB_OD:CB_OD + 128], sq[:, c, :], c == 0, c == 7, ["sq", "cbf"], [("ps", b)])
                actf(rs, psum[b][:], AF.Sqrt, [("ps", b)], ["rs"], bias=epsb[:, 0:1])
                recip(rs, rs, ["rs"], ["rs"])
                for c in range(8):
                    stt(hT[:, c, s], xT[:, c, s], vcol(l, goff + c), rs, ALU.mult, ALU.mult,
                        [("x", c, tc), "rs", "vec"], HR(tc))
                if hf is not None:
                    hf(tc, rs)

        def proj(wt, tc, b, wkey):
            s = tsl(tc)
            for kc in range(8):
                mm(psum[b][:], wt[:, kc, :], hT[:, kc, s], kc == 0, kc == 7, [wkey] + HR(tc), [("ps", b)])

        def wblk(ar):
            return ar.bf(1024)

        def w3(t):
            return t.rearrange("p (k c) -> p k c", k=8)

        ar0 = Ar()
        epsb = ar0.f32(2)
        P.op("dve", lambda e: e.memset(epsb[:, 0:1], EPS), (), ["epsb"])
        P.op("dve", lambda e: e.memset(epsb[:, 1:2], 1e-5), (), ["epsb"])
        base_off = ar0.off

        dump_state = {"off": 0}

        def dump_tile(ap_f32_or_bf, n, reads, ar):
            tmp = arena[:, ARN - 1024:ARN].bitcast(F32)
            P.op("dve", lambda e: e.tensor_copy(out=tmp, in_=ap_f32_or_bf), reads, ["dumptmp"])
            o = dump_state["off"]
            P.dma("sp", dump_d[:, o:o + n], tmp, "dumpst", reads=["dumptmp"], writes=["dumpout%d" % o])
            dump_state["off"] += n

        for bat, l in [(bb_, l_) for bb_ in range(nbatch) for l_ in layers]:
            if l == layers[0]:
                for c in range(8):
                    P.dma("sp", xT[:, c, :], xT_d[bat, c * 128:(c + 1) * 128, :], ("ld_x", c), writes=[("x", c, tc) for tc in range(4)])
            ar = Ar()
            ar.off = base_off
            merged = ar.bf(8 * T).rearrange("p (c t) -> p c t", c=8)
            zbuf = ar.bf(4 * T).rearrange("p (c t) -> p c t", c=4)
            work_off = ar.off

            P.barrier()
            arn = Ar(); arn.off = work_off
            rmsnorm(l, V_GM, arn)

            def branch_out(br, first):
                P.barrier()
                a = Ar(); a.off = work_off
                wo = [a.bf(512) for _ in range(2)]
                wg = [a.bf(1024) for _ in range(2)]
                sg = a.bf(512)
                tmp = a.bf(512)
                for oc in range(8):
                    k = oc % 2
                    wload(wo[k], w_bo_d[l, br, oc], ("wo", k))
                    wload(wg[k], w_in_d[l, 36 + br * 8 + oc], ("wg", k))
                    wo3 = wo[k].rearrange("p (k c) -> p k c", k=4)
                    for tc in range(NTC):
                        s = tsl(tc)
                        by = bank()
                        for kc in range(4):
                            mm(psum[by][:], wo3[:, kc, :], zbuf[:, kc, s], kc == 0, kc == 3, [("wo", k), ("z", tc)], [("ps", by)])
                        bg = bank()
                        proj(w3(wg[k]), tc, bg, ("wg", k))
                        actf(sg, psum[bg][:], AF.Sigmoid, [("ps", bg)], ["sg"])
                        if first:
                            tt("dve", merged[:, oc, s], psum[by][:], sg, ALU.mult, [("ps", by), "sg"], [("mg", oc, tc)])
                        else:
                            tt("dve", tmp, psum[by][:], sg, ALU.mult, [("ps", by), "sg"], ["botmp"])
                            tt("pool", merged[:, oc, s], merged[:, oc, s], tmp, ALU.add, ["botmp", ("mg", oc, tc)], [("mg", oc, tc)])

            ZW = lambda tc: [("z", tc)]

            if "ret" in phases:
                P.barrier()
                a = Ar(); a.off = work_off
                P.dma("pool", rope[:].rearrange("p a (b t) -> p (a b) t", b=2), rope_d[0].rearrange("p (a t) -> p a t", a=4), "ld_rope", writes=["rope"])
                wq, wk, wv, wgt = wblk(a), wblk(a), wblk(a), wblk(a)
                qT = a.bf(T); kT = a.bf(T); gT = a.bf(T)
                vtm = a.bf(T).rearrange("p (i e) -> p i e", i=16)
                qsb = a.bf(512)
                t1 = a.f32(512); t2 = a.f32(512)
                sT = a.bf(128); q2 = a.bf(128); ktm = a.bf(128)
                S_f = a.f32(128); S_b = a.bf(128)
                ysb = a.bf(512); ysq = a.bf(512)
                for h in range(4 if RS >= 5 else 1):
                    wload(wq, w_in_d[l, h], "wq"); wload(wk, w_in_d[l, 4 + h], "wk")
                    wload(wv, w_in_d[l, 8 + h], "wv"); wload(wgt, w_in_d[l, 12 + h], "wgt")
                    for tc in range(NTC if RS >= 2 else 0):
                        s = tsl(tc)
                        for (wt, wkey, dst, dkey, scl) in ((wq, "wq", qT, "qT", 1.0), (wk, "wk", kT, "kT", 128.0 ** -0.5)):
                            b = bank()
                            proj(w3(wt), tc, b, wkey)
                            actf(qsb, psum[b][:], AF.Copy, [("ps", b)], ["qsb"])
                            if R2 < 2:
                                continue
                            b2 = bank()
                            if R3 != 2:
                                mm(psum[b2][:], cbf[:, CB_RR:CB_RR + 128], qsb, True, True, ["qsb", "cbf"], [("ps", b2)])
                            if R3 != 1:
                                stt(t1, psum[b][:], scl, rope[:, 0, s], ALU.mult, ALU.mult, [("ps", b), "rope", "qsb"], ["t1"])
                            if R3 == 0:
                                stt(t2, psum[b2][:], scl, rope[:, 1, s], ALU.mult, ALU.mult, [("ps", b2), "rope"], ["t2"])
                            if R2 < 3:
                                continue
                            tt("pool", dst[:, s], t1, t2, ALU.add, ["t1", "t2"], [(dkey, tc)])
                        if R2 < 4:
                            continue
                        b = bank()
                        proj(w3(wgt), tc, b, "wgt")
                        actf(gT[:, s], psum[b][:], AF.Silu, [("ps", b)], [("gT", tc)])
                        if R2 < 5:
                            continue
                        b = bank()
                        for i in range(4):
                            ti = tc * 4 + i
                            for kc in range(8):
                                mm(psum[b][:, i * 128:(i + 1) * 128], hT[:, kc, ti * 128:(ti + 1) * 128], w3(wv)[:, kc, :],
                                   kc == 0, kc == 7, ["wv"] + HR(tc), [("ps", b)])
                        actf(vtm[:, tc * 4:(tc + 1) * 4, :], psum[b][:].rearrange("p (i e) -> p i e", i=4), AF.Copy, [("ps", b)], [("vtm", tc)])
                    gC = float((1.0 - 2.0 ** (-5.0 - h)) ** 128)
                    by = None
                    for c in range(16 if RS >= 3 else 0):
                        tc = c // 4
                        cs = slice(c * 128, (c + 1) * 128)
                        bs = bank()
                        mm(psum[bs][:, 0:128], kT[:, cs], qT[:, cs], True, True, [("kT", tc), ("qT", tc)], [("ps", bs)])
                        tt("dve", sT, psum[bs][:, 0:128], cf[:, CF_DEC + h * 128:CF_DEC + (h + 1) * 128], ALU.mult, [("ps", bs), "cf"], ["sT"])
                        if c > 0:
                            tt("pool", q2, qT[:, cs], cf[:, CF_WCB + h * 128:CF_WCB + (h + 1) * 128], ALU.mult, [("qT", tc), "cf"], ["q2"])
                        if c % 4 == 0:
                            by = bank(hold=True)
                        ysl = psum[by][:, (c % 4) * 128:(c % 4 + 1) * 128]
                        mm(ysl, vtm[:, c, :], sT, True, c == 0, [("vtm", tc), "sT"], [("ps", by)])
                        if c > 0:
                            mm(ysl, S_b, q2, False, True, ["S_b", "q2"], [("ps", by)])
                        if c < 15:
                            bt = bank()
                            ptr = psum[bt][:, 0:64].bitcast(BF16)
                            tr(ptr, kT[:, cs], [("kT", tc)], [("ps", bt)])
                            ts("dve", ktm, ptr, cf[:, CF_WS + h:CF_WS + h + 1], None, ALU.mult, None, [("ps", bt), "cf"], ["ktm"])
                            bk = bank()
                            mm(psum[bk][:, 0:128], ktm, vtm[:, c, :], True, True, ["ktm", ("vtm", tc)], [("ps", bk)])
                            if c == 0:
                                P.op("dve", lambda e, bk=bk: e.tensor_copy(out=S_f, in_=psum[bk][:, 0:128]), [("ps", bk)], ["S_f"])
                            else:
                                stt(S_f, S_f, gC, psum[bk][:, 0:128], ALU.mult, ALU.add, [("ps", bk), "S_f"], ["S_f"])
                            actf(S_b, S_f, AF.Copy, ["S_f"], ["S_b"])
                        if c % 4 == 3 and RS < 4:
                            held.discard(by)
                        if c % 4 == 3 and RS >= 4:
                            s = tsl(tc)
                            actf(ysb, psum[by][:], AF.Copy, [("ps", by)], ["ysb"])
                            actf(ysq, psum[by][:], AF.Square, [("ps", by)], ["ysq"])
                            bm = bank(); bq = bank()
                            mm(psum[bm][:], cbf[:, CB_O128:CB_O128 + 128], ysb, True, True, ["ysb", "cbf"], [("ps", bm)])
                            mm(psum[bq][:], cbf[:, CB_O128:CB_O128 + 128], ysq, True, True, ["ysq", "cbf"], [("ps", bq)])
                            actf(t1, psum[bm][:], AF.Square, [("ps", bm)], ["t1"])
                            tt("dve", t1, psum[bq][:], t1, ALU.subtract, [("ps", bq), "t1"], ["t1"])
                            actf(t1, t1, AF.Sqrt, ["t1"], ["t1"], bias=epsb[:, 1:2])
                            recip(t1, t1, ["t1"], ["t1"])
                            actf(t2, psum[bm][:], AF.Copy, [("ps", bm)], ["t2"])
                            tt("dve", t2, psum[by][:], t2, ALU.subtract, [("ps", by), "t2"], ["t2"])
                            tt("dve", t2, t2, t1, ALU.mult, ["t1", "t2"], ["t2"])
                            tt("dve", zbuf[:, h, s], t2, gT[:, s], ALU.mult, ["t2", ("gT", tc)], ZW(tc))
                            held.discard(by)
                if dump is not None and dump_state["off"] == 0:
                    dump_tile(zbuf[:, 0, 0:512], 512, ZW(0), a)
                    dump_tile(hT[:, 0, 0:512], 512, HR(0), a)
                    dump_tile(qT[:, 0:512], 512, [("qT", 0)], a)
                if RS >= 6:
                    branch_out(0, True)

            if "conv" in phases:
                P.barrier()
                a = Ar(); a.off = work_off
                wca, wcb_ = wblk(a), wblk(a)
                upad = a.bf(30 + T + 2)
                dg = a.bf(31 * 128).rearrange("p (j c) -> p j c", j=31)
                sgb = a.f32(512)
                for cc in range(4):
                    wload(wca, w_in_d[l, 16 + cc], "wca"); wload(wcb_, w_in_d[l, 20 + cc], "wcb")
                    P.op("pool", lambda e: e.memset(upad[:, 0:30], 0.0), (), [("up", -1)])
                    for tc in range(NTC):
                        ba = bank(); proj(w3(wca), tc, ba, "wca")
                        bb = bank(); proj(w3(wcb_), tc, bb, "wcb")
                        actf(sgb, psum[bb][:], AF.Sigmoid, [("ps", bb)], ["sgb"])
                        tt("dve", upad[:, 30 + tc * 512:30 + (tc + 1) * 512], psum[ba][:], sgb, ALU.mult, [("ps", ba), "sgb"], [("up", tc)])
                    for j in range(31):
                        ts("dve", dg[:, j, :], ident(), vcol(l, V_CW + cc * 31 + j), None, ALU.mult, None, ["cbf", "vec"], [("dg", j)])
                    for tc in range(NTC):
                        s = tsl(tc)
                        b = bank()
                        for j in range(31):
                            mm(psum[b][:], dg[:, j, :], upad[:, tc * 512 + j:tc * 512 + j + 512], j == 0, j == 30,
                               [("dg", j), ("up", tc), ("up", tc - 1)], [("ps", b)])
                        actf(zbuf[:, cc, s], psum[b][:], AF.Identity, [("ps", b)], ZW(tc), bias=vcol(l, V_CB + cc))
                P.barrier()
                a2 = Ar(); a2.off = work_off
                csq = a2.bf(4 * 512).rearrange("p (c t) -> p c t", c=4)
                m_sb = a2.f32(512); r_sb = a2.f32(512); tq = a2.f32(512)
                for tc in range(NTC):
                    s = tsl(tc)
                    actf(csq, zbuf[:, :, s], AF.Square, ZW(tc), ["csq"])
                    bm = bank(); bq = bank()
                    for cc in range(4):
                        mm(psum[bm][:], cbf[:, CB_O512:CB_O512 + 128], zbuf[:, cc, s], cc == 0, cc == 3, ZW(tc) + ["cbf"], [("ps", bm)])
                    for cc in range(4):
                        mm(psum[bq][:], cbf[:, CB_O512:CB_O512 + 128], csq[:, cc, :], cc == 0, cc == 3, ["csq", "cbf"], [("ps", bq)])
                    actf(m_sb, psum[bm][:], AF.Copy, [("ps", bm)], ["m_sb"])
                    actf(r_sb, psum[bm][:], AF.Square, [("ps", bm)], ["r_sb"])
                    tt("dve", r_sb, psum[bq][:], r_sb, ALU.subtract, [("ps", bq), "r_sb"], ["r_sb"])
                    actf(r_sb, r_sb, AF.Sqrt, ["r_sb"], ["r_sb"], bias=epsb[:, 0:1])
                    recip(r_sb, r_sb, ["r_sb"], ["r_sb"])
                    for cc in range(4):
                        tt("dve", tq, zbuf[:, cc, s], m_sb, ALU.subtract, ZW(tc) + ["m_sb"], ["tq"])
                        tt("dve", tq, tq, r_sb, ALU.mult, ["tq", "r_sb"], ["tq"])
                        actf(zbuf[:, cc, s], tq, AF.Silu, ["tq", "vec"], ZW(tc), scale=vcol(l, V_LG + cc), bias=vcol(l, V_LB + cc))
                if dump is not None and dump_state["off"] == 1536:
                    dump_tile(zbuf[:, 0, 0:512], 512, ZW(0), a2)
                branch_out(1, False)

            if "moba" in phases:
                P.barrier()
                a = Ar(); a.off = work_off
                P.dma("pool", rope[:].rearrange("p a (b t) -> p (a b) t", b=2), rope_d[1].rearrange("p (a t) -> p a t", a=4), "ld_rope", writes=["rope"])
                wq, wk, wv = wblk(a), wblk(a), wblk(a)
                qT = a.bf(T); kT = a.bf(T)
                vaug = a.bf(16 * 2 * 66).rearrange("p (i h e) -> p i h e", i=16, h=2)
                attn = a.bf(16 * 128).rearrange("p (i e) -> p i e", i=16)
                sqb = a.bf(512); qnb = a.bf(512)
                rsd = a.f32(512); qn = a.f32(512); t1 = a.f32(512); t2 = a.f32(512)
                km = a.f32(8); kmb = a.bf(8)
                gsb = a.f32(256); top8 = a.f32(256); sel = a.f32(256)
                ex = [a.bf(256), a.bf(256)]
                acc = a.f32(66); rcp = a.f32(2)
                P.op("pool", lambda e: e.memset(vaug[:, :, :, 64:65], 1.0), (), ["vones"])
                for hp in range(4):
                    wload(wq, w_in_d[l, 24 + hp], "wq"); wload(wk, w_in_d[l, 28 + hp], "wk"); wload(wv, w_in_d[l, 32 + hp], "wv")
                    for tc in range(NTC):
                        s = tsl(tc)
                        for (wt, wkey, dst, dkey, goff) in ((wq, "wq", qT, "qT", V_QG), (wk, "wk", kT, "kT", V_KG)):
                            b = bank()
                            proj(w3(wt), tc, b, wkey)
                            actf(sqb, psum[b][:], AF.Square, [("ps", b)], ["sqb"])
                            b2 = bank()
                            mm(psum[b2][:], cbf[:, CB_B64:CB_B64 + 128], sqb, True, True, ["sqb", "cbf"], [("ps", b2)])
                            actf(rsd, psum[b2][:], AF.Sqrt, [("ps", b2)], ["rsd"], bias=epsb[:, 0:1])
                            recip(rsd, rsd, ["rsd"], ["rsd"])
                            stt(qn, psum[b][:], vcol(l, goff), rsd, ALU.mult, ALU.mult, [("ps", b), "rsd", "vec"], ["qn"])
                            actf(qnb, qn, AF.Copy, ["qn"], ["qnb"])
                            b3 = bank()
                            mm(psum[b3][:], cbf[:, CB_RM:CB_RM + 128], qnb, True, True, ["qnb", "cbf"], [("ps", b3)])
                            tt("dve", t1, qn, rope[:, 0, s], ALU.mult, ["qn", "rope"], ["t1"])
                            tt("dve", t2, psum[b3][:], rope[:, 1, s], ALU.mult, [("ps", b3), "rope"], ["t2"])
                            tt("pool", dst[:, s], t1, t2, ALU.add, ["t1", "t2"], [(dkey, tc)])
                        b = bank()
                        for i in range(4):
                            ti = tc * 4 + i
                            for kc in range(8):
                                mm(psum[b][:, i * 128:(i + 1) * 128], hT[:, kc, ti * 128:(ti + 1) * 128], w3(wv)[:, kc, :],
                                   kc == 0, kc == 7, ["wv"] + HR(tc), [("ps", b)])
                        actf(vaug[:, tc * 4:(tc + 1) * 4, :, 0:64], psum[b][:].rearrange("p (i h e) -> p i h e", i=4, h=2), AF.Copy,
                             [("ps", b), "vones"], [("va", tc)])
                    KA = [("kT", tc) for tc in range(4)]
                    QA = [("qT", tc) for tc in range(4)]
                    P.op("dve", lambda e: e.tensor_reduce(out=km, in_=kT.rearrange("p (b s) -> p b s", s=256), axis=AX.X, op=ALU.add), KA, ["km"])
                    actf(kmb, km, AF.Copy, ["km"], ["kmb"], scale=1.0 / 256.0)
                    for hh in range(2):
                        hs = slice(hh * 64, (hh + 1) * 64)
                        bg = bank()
                        for i in range(16):
                            mm(psum[bg][:, i * 8:(i + 1) * 8], qT[hs, i * 128:(i + 1) * 128], kmb[hs, :], True, True,
                               QA + ["kmb"], [("ps", bg)])
                        tt("dve", gsb[:, 0:128], psum[bg][:, 0:128], cf[:, CF_PNEG:CF_PNEG + 128], ALU.add, [("ps", bg), "cf"], ["gsb"])
                        for i in range(2, 16):
                            P.op("dve", lambda e, i=i: e.max(out=top8[:, i * 8:(i + 1) * 8], in_=gsb[:, i * 8:(i + 1) * 8]), ["gsb"], ["top8"])
                            ts("dve", sel[:, i * 8:(i + 1) * 8], gsb[:, i * 8:(i + 1) * 8], top8[:, i * 8 + 2:i * 8 + 3], None, ALU.is_ge, None,
                               ["gsb", "top8"], ["sel"])
                        tt("dve", sel[:, 16:128], sel[:, 16:128], cf[:, CF_PMSK + 16:CF_PMSK + 128], ALU.mult, ["sel", "cf"], ["sel"])
                        for qb in range(8):
                            qsl = slice(qb * 256, (qb + 1) * 256)
                            qtc = qb // 2
                            bo = [bank(hold=True), bank(hold=True)]
                            bz = bank(hold=True)
                            for kb in range(qb + 1):
                                for lc in range(2):
                                    lsl = slice(kb * 256 + lc * 128, kb * 256 + (lc + 1) * 128)
                                    bsx = bank()
                                    mm(psum[bsx][:, 0:256], kT[hs, lsl], qT[hs, qsl], True, True, [("kT", kb // 2), ("qT", qtc)], [("ps", bsx)])
                                    e_ = ex[lc]
                                    ek = ("ex", lc)
                                    actf(e_, psum[bsx][:, 0:256], AF.Exp, [("ps", bsx)], [ek], scale=0.125)
                                    own = kb == qb
                                    if own:
                                        dsl = slice(lc * 128, (lc + 1) * 128)
                                        tt("pool", e_[:, dsl], e_[:, dsl], cbf[:, CB_TRI:CB_TRI + 128], ALU.mult, [ek, "cbf"], [ek])
                                    for qt in range(2):
                                        if own and lc == 1 and qt == 0:
                                            continue
                                        if own:
                                            osl = psum[bz][:, qt * 65:(qt + 1) * 65]
                                            okey = ("ps", bz)
                                            st = (lc == 0) or (qt == 0)
                                            if qt == 1:
                                                st = lc == 0
                                            sp_ = (lc == 1) or (qt == 0)
                                        else:
                                            osl = psum[bo[qt]][:, kb * 65:(kb + 1) * 65]
                                            okey = ("ps", bo[qt])
                                            st = lc == 0
                                            sp_ = lc == 1
                                        mm(osl, e_[:, qt * 128:(qt + 1) * 128], vaug[:, kb * 2 + lc, hh, 0:65], st, sp_,
                                           [ek, ("va", kb // 2), "vones"], [okey])
                            for qt in range(2):
                                ti = qb * 2 + qt
                                P.op("dve", lambda e, bz=bz, qt=qt: e.tensor_copy(out=acc[:, 0:65], in_=psum[bz][:, qt * 65:(qt + 1) * 65]), [("ps", bz)], ["acc"])
                                for kb in range(qb):
                                    stt(acc[:, 0:65], psum[bo[qt]][:, kb * 65:(kb + 1) * 65], sel[:, ti * 8 + kb:ti * 8 + kb + 1], acc[:, 0:65],
                                        ALU.mult, ALU.add, [("ps", bo[qt]), "sel", "acc"], ["acc"])
                                recip(rcp[:, 0:1], acc[:, 64:65], ["acc"], ["rcp"])
                                ts("dve", attn[:, ti, hs], acc[:, 0:64], rcp[:, 0:1], None, ALU.mult, None, ["acc", "rcp"], [("attn", ti)])
                            held.discard(bo[0]); held.discard(bo[1]); held.discard(bz)
                    for tc in range(NTC):
                        bt = bank()
                        pt = psum[bt][:, 0:256].bitcast(BF16)
                        for i in range(4):
                            ti = tc * 4 + i
                            tr(pt[:, i * 128:(i + 1) * 128], attn[:, ti, :], [("attn", ti)], [("ps", bt)])
                        actf(zbuf[:, hp, tsl(tc)], pt, AF.Copy, [("ps", bt)], ZW(tc))
                if dump is not None and dump_state["off"] == 2048:
                    dump_tile(zbuf[:, 0, 0:512], 512, ZW(0), a)
                    dump_tile(zbuf[:, 3, 1536:2048], 512, ZW(3), a)
                branch_out(2, False)

            if "merge" in phases:
                P.barrier()
                a = Ar(); a.off = work_off
                wo_ = [wblk(a), wblk(a)]
                for oc in range(8):
                    k = oc % 2
                    wload(wo_[k], w_out_d[l, oc], ("wout", k))
                    for tc in range(NTC):
                        s = tsl(tc)
                        b = bank()
                        for kc in range(8):
                            mm(psum[b][:], w3(wo_[k])[:, kc, :], merged[:, kc, s], kc == 0, kc == 7, [("wout", k), ("mg", kc, tc)], [("ps", b)])
                        tt("dve", xT[:, oc, s], psum[b][:], xT[:, oc, s], ALU.add, [("ps", b), ("x", oc, tc)], [("x", oc, tc)])
                if dump is not None and dump_state["off"] == 3072:
                    dump_tile(xT[:, 0, 0:512], 512, [("x", 0, 0)], a)

            if "ffn" in phases:
                P.barrier()
                a = Ar(); a.off = base_off
                j = l // 2
                moe = (l % 2 == 1)
                if moe:
                    wr = a.f32(64)
                    P.dma("sp", wr, w_rt_d[j], "ld_wr", writes=["wr"])
                    hf = a.f32(8 * 512).rearrange("p (c t) -> p c t", c=8)
                    lg = a.f32(128)
                    gw = a.f32(128)
                    gt8 = a.f32(T)
                    tp8 = a.f32(8); nm1 = a.f32(1); dn = a.f32(1)

                    def hf_fn(tc, rs):
                        s = tsl(tc)
                        for c in range(8):
                            stt(hf[:, c, :], xT[:, c, s], vcol(l, V_GF + c), rs, ALU.mult, ALU.mult, [("x", c, tc), "rs", "vec"], ["hf"])
                        bl = bank()
                        for i in range(4):
                            for kc in range(8):
                                mm(psum[bl][:, i * 8:(i + 1) * 8], hf[:, kc, i * 128:(i + 1) * 128], wr[:, kc * 8:(kc + 1) * 8], kc == 0, kc == 7,
                                   ["hf", "wr"], [("ps", bl)])
                        P.op("dve", lambda e, bl=bl, tc=tc: e.tensor_copy(out=lg[:, tc * 32:(tc + 1) * 32], in_=psum[bl][:, 0:32]), [("ps", bl)], ["lg"])

                    arn = Ar(); arn.off = a.off
                    rmsnorm(l, V_GF, arn, hf=hf_fn)
                    a.off = arn.off
                    for i in range(16):
                        lsl = slice(i * 8, (i + 1) * 8)
                        P.op("dve", lambda e, lsl=lsl: e.max(out=tp8, in_=lg[:, lsl]), ["lg"], ["tp8"])
                        ts("dve", nm1, tp8[:, 0:1], -1.0, None, ALU.mult, None, ["tp8"], ["nm1"])
                        actf(gw[:, lsl], lg[:, lsl], AF.Exp, ["lg", "nm1"], ["gw"], bias=nm1[:, 0:1])
                        stt(gw[:, lsl], lg[:, lsl], tp8[:, 1:2], gw[:, lsl], ALU.is_ge, ALU.mult, ["lg", "tp8", "gw"], ["gw"])
                        P.op("dve", lambda e, lsl=lsl: e.tensor_reduce(out=dn, in_=gw[:, lsl], axis=AX.X, op=ALU.add), ["gw"], ["dn"])
                        recip(dn, dn, ["dn"], ["dn"])
                        ts("dve", gw[:, lsl], gw[:, lsl], dn[:, 0:1], None, ALU.mult, None, ["gw", "dn"], ["gw"])
                    for tc in range(NTC):
                        bt = bank()
                        for i in range(4):
                            ti = tc * 4 + i
                            P.op("pe", lambda e, bt=bt, i=i, ti=ti: e.transpose(psum[bt][0:8, i * 128:(i + 1) * 128], gw[:, ti * 8:(ti + 1) * 8],
                                                                              cf[:, CF_IDF:CF_IDF + 128]), ["gw", "cf"], [("ps", bt)])
                        actf(gt8[0:8, tsl(tc)], psum[bt][0:8, :], AF.Copy, [("ps", bt)], ["gt8"])
                    gwb = a.bf(T)
                    ne, nfc = 8, 28
                else:
                    arn = Ar(); arn.off = a.off
                    rmsnorm(l, V_GF, arn)
                    a.off = arn.off
                    ne, nfc = 1, 22
                wgs = [a.bf(2048), a.bf(2048)]
                wus = [a.bf(2048), a.bf(2048)]
                wds = [a.bf(2048), a.bf(2048)]
                act_ = [a.bf(1024), a.bf(1024)]
                sil = a.bf(512); sil2 = a.bf(512)
                gi = 0
                for e_i in range(ne):
                    if moe:
                        for tc in range(NTC):
                            b = bank()
                            mm(psum[b][:], cf[0:8, CF_SEL8 + e_i * 128:CF_SEL8 + (e_i + 1) * 128], gt8[0:8, tsl(tc)], True, True, ["gt8", "cf"], [("ps", b)])
                            actf(gwb[:, tsl(tc)], psum[b][:], AF.Copy, [("ps", b)], ["gwb"])
                    for fg in range(nfc // 2):
                        k = gi % 2
                        gi += 1
                        if moe:
                            sg_, su_, sd_ = w_eg_d[j, e_i], w_eu_d[j, e_i], w_ed_d[j, e_i]
                        else:
                            sg_, su_, sd_ = w_fg_d[j], w_fu_d[j], w_fd_d[j]
                        src = lambda d: d[2 * fg:2 * fg + 2].rearrange("f p n -> p f n")
                        wload(wgs[k].rearrange("p (f n) -> p f n", f=2), src(sg_), ("wfg", k))
                        wload(wus[k].rearrange("p (f n) -> p f n", f=2), src(su_), ("wfu", k))
                        wload(wds[k].rearrange("p (f n) -> p f n", f=2), src(sd_), ("wfd", k))
                        for tc in range(NTC):
                            s = tsl(tc)
                            ak = act_[(gi * 4 + tc) % 2]
                            akey = ("actb", (gi * 4 + tc) % 2)
                            for fi in range(2):
                                bg_ = bank()
                                proj(w3(wgs[k][:, fi * 1024:(fi + 1) * 1024]), tc, bg_, ("wfg", k))
                                bu_ = bank()
                                proj(w3(wus[k][:, fi * 1024:(fi + 1) * 1024]), tc, bu_, ("wfu", k))
                                actf(sil, psum[bg_][:], AF.Silu, [("ps", bg_)], ["sil"])
                                if moe:
                                    tt("pool", sil2, sil, gwb[:, s], ALU.mult, ["sil", "gwb"], ["sil2"])
                                    tt("dve", ak[:, fi * 512:(fi + 1) * 512], psum[bu_][:], sil2, ALU.mult, [("ps", bu_), "sil2"], [akey])
                                else:
                                    tt("dve", ak[:, fi * 512:(fi + 1) * 512], psum[bu_][:], sil, ALU.mult, [("ps", bu_), "sil"], [akey])
                            for oc in range(8):
                                bd = bank()
                                for fi in range(2):
                                    mm(psum[bd][:], wds[k][:, fi * 1024 + oc * 128:fi * 1024 + (oc + 1) * 128], ak[:, fi * 512:(fi + 1) * 512],
                                       fi == 0, fi == 1, [("wfd", k), akey], [("ps", bd)])
                                tt("dve", xT[:, oc, s], psum[bd][:], xT[:, oc, s], ALU.add, [("ps", bd), ("x", oc, tc)], [("x", oc, tc)])

            if l == layers[-1]:
                for c in range(8):
                    P.dma("sp", out_d[bat, c * 128:(c + 1) * 128, :], xT[:, c, :], ("st_out", c),
                          reads=[("x", c, tc) for tc in range(4)], writes=[("outd", bat, c)])

        if dump is not None:
            for o in range(0, dump_state["off"], 512):
                r = P.res.get("dumpout%d" % o)
                if r is not None:
                    P.wait_tok("sp", r["w"])

        P.barrier()
        engs = {"pe": nc.tensor, "act": nc.scalar, "dve": nc.vector, "pool": nc.gpsimd, "sp": nc.sync}

        def emit_all():
            for k in ENGS:
                P.emit(k, engs[k])

        emit_all()
    return nc


def _consts():
    cb = np.zeros((128, CB_N), np.float32)
    cb[:, CB_ID:CB_ID + 128] = np.eye(128)
    cb[:, CB_OD:CB_OD + 128] = 1.0 / 1024
    cb[:, CB_O512:CB_O512 + 128] = 1.0 / 512
    cb[:, CB_O128:CB_O128 + 128] = 1.0 / 128
    for g in range(2):
        cb[g * 64:(g + 1) * 64, CB_B64 + g * 64:CB_B64 + (g + 1) * 64] = 1.0 / 64
    R = np.zeros((128, 128), np.float32)
    for jj in range(64):
        R[jj + 64, jj] = -1.0
        R[jj, jj + 64] = 1.0
    cb[:, CB_RR:CB_RR + 128] = R
    R = np.zeros((128, 128), np.float32)
    for g in range(2):
        for jj in range(32):
            R[g * 64 + jj + 32, g * 64 + jj] = -1.0
            R[g * 64 + jj, g * 64 + jj + 32] = 1.0
    cb[:, CB_RM:CB_RM + 128] = R
    li = np.arange(128)
    cb[:, CB_TRI:CB_TRI + 128] = (li[:, None] <= li[None, :]).astype(np.float32)

    def tables(d):
        inv = (1.0 / (np.float32(10000.0) ** (np.arange(0, d, 2, dtype=np.float32) / np.float32(d)))).astype(np.float32)
        ang = (np.arange(T, dtype=np.float32)[:, None] * inv[None, :]).astype(np.float32)
        return np.cos(ang).astype(np.float32), np.sin(ang).astype(np.float32)

    rope = np.zeros((2, 128, 2 * T), np.float32)
    c, s = tables(128)
    for p in range(128):
        rope[0, p, :T] = c[:, p % 64]
        rope[0, p, T:] = s[:, p % 64]
    c, s = tables(64)
    for p in range(128):
        rope[1, p, :T] = c[:, p % 32]
        rope[1, p, T:] = s[:, p % 32]

    cf = np.zeros((128, CF_N), np.float32)
    pos = np.arange(128, dtype=np.float64)
    for h in range(4):
        g = 1.0 - 2.0 ** (-5.0 - h)
        lgv = np.log(g)
        diff = pos[None, :] - pos[:, None]
        dec = np.where(diff >= 0, np.exp(diff * lgv), 0.0)
        cf[:, CF_DEC + h * 128:CF_DEC + (h + 1) * 128] = dec
        cf[:, CF_WCB + h * 128:CF_WCB + (h + 1) * 128] = np.exp((pos + 1.0) * lgv)[None, :]
        cf[:, CF_WS + h] = np.exp((127.0 - pos) * lgv)
    cf[:, CF_IDF:CF_IDF + 128] = np.eye(128)
    for i in range(16):
        qb = i // 2
        for b in range(8):
            cf[:, CF_PNEG + i * 8 + b] = 0.0 if b < qb else -1e30
            cf[:, CF_PMSK + i * 8 + b] = 1.0 if b < qb else 0.0
    for e in range(8):
        cf[e, CF_SEL8 + e * 128:CF_SEL8 + (e + 1) * 128] = 1.0
    return cb, rope, cf


def _blk(w, n):
    k = w.shape[0] // 128
    return np.ascontiguousarray(w.reshape(k, 128, n, 128).transpose(2, 1, 0, 3).reshape(n, 128, k * 128))


def _prep(inp):
    f = lambda a: np.asarray(a, dtype=np.float32)
    w_in = np.stack([_blk(f(inp["w_in"][l]), 60) for l in range(L)])
    w_bo = np.stack([np.stack([_blk(f(inp[n][l]), 8) for n in ("w_ret_o", "w_conv_o", "w_moba_o")]) for l in range(L)])
    w_out = np.stack([_blk(f(inp["w_out"][l]), 8) for l in range(L)])
    w_fg = np.stack([_blk(f(inp["w_ff_gate"][j]), 22) for j in range(2)])
    w_fu = np.stack([_blk(f(inp["w_ff_up"][j]), 22) for j in range(2)])
    w_fd = np.ascontiguousarray(f(inp["w_ff_down"]).reshape(2, 22, 128, 1024))
    w_eg = np.stack([np.stack([_blk(f(inp["w_e_gate"][j, e]), 28) for e in range(8)]) for j in range(2)])
    w_eu = np.stack([np.stack([_blk(f(inp["w_e_up"][j, e]), 28) for e in range(8)]) for j in range(2)])
    w_ed = np.ascontiguousarray(f(inp["w_e_down"]).reshape(2, 8, 28, 128, 1024))
    w_rt = np.ascontiguousarray(f(inp["w_router"]).reshape(2, 8, 128, 8).transpose(0, 2, 1, 3).reshape(2, 128, 64))
    vec = np.zeros((128, L * V_N), np.float32)
    for l in range(L):
        o = l * V_N
        vec[:, o + V_GM:o + V_GM + 8] = f(inp["g_mix"][l]).reshape(8, 128).T
        vec[:, o + V_GF:o + V_GF + 8] = f(inp["g_ffn"][l]).reshape(8, 128).T
        cw = f(inp["conv_w"][l])
        vec[:, o + V_CW:o + V_CW + 124] = cw.reshape(31, 4, 128).transpose(2, 1, 0).reshape(128, 124)
        vec[:, o + V_CB:o + V_CB + 4] = f(inp["conv_b"][l]).reshape(4, 128).T
        vec[:, o + V_LG:o + V_LG + 4] = f(inp["conv_ln_g"][l]).reshape(4, 128).T
        vec[:, o + V_LB:o + V_LB + 4] = f(inp["conv_ln_b"][l]).reshape(4, 128).T
        vec[:, o + V_QG] = np.tile(f(inp["q_norm_g"][l]), 2)
        vec[:, o + V_KG] = np.tile(f(inp["k_norm_g"][l]), 2)
    cb, rope, cf = _consts()
    return dict(w_in=w_in, w_bo=w_bo, w_out=w_out, w_fg=w_fg, w_fu=w_fu, w_fd=w_fd, w_eg=w_eg, w_eu=w_eu,
                w_ed=w_ed, w_rt=w_rt, vec=vec, cbf=cb, rope=rope, cf=cf)


def kernel(**inputs):
    x = np.asarray(inputs["x"], dtype=np.float32)
    shared = _prep(inputs)
    ncores, per = 4, 2
    nc = build_nc(nbatch=per)
    in_maps = []
    for k in range(ncores):
        m = dict(shared)
        m["xT"] = np.ascontiguousarray(x[k * per:(k + 1) * per].transpose(0, 2, 1))
        in_maps.append(m)
    res = run_bass_kernel_spmd(nc, in_maps, core_ids=list(range(ncores)))
    o = np.concatenate([np.asarray(r["outT"]) for r in res.results], axis=0)
    return np.ascontiguousarray(o.transpose(0, 2, 1)).astype(np.float32)
```
